# Optimizing a Trainium2 kernel written in Bass

```python
import math
import jax
import jax.numpy as jnp
from jax import lax
import numpy as np

D_MODEL = 1024
BATCH = 4
SEQ = 8192
DEPTH = 2

CHUNK = 64
Q_BLOCK = 128
ROPE_THETA = 500000.0
HEAD_DIM = 64

MLA_HEADS = 8
DIFF_HEADS = 4
FOX_HEADS = 4
D_MIX = (MLA_HEADS + DIFF_HEADS + FOX_HEADS) * HEAD_DIM

MLA_Q_RANK = 384
MLA_KV_RANK = 256
MLA_NOPE = 64
MLA_ROPE = 32
MLA_V = HEAD_DIM

DIFF_QK = HEAD_DIM // 2
DIFF_V = HEAD_DIM
DIFF_ROT = DIFF_QK // 4
DIFF_W = DIFF_HEADS * HEAD_DIM

FOX_W = FOX_HEADS * HEAD_DIM

IN_SPLITS = (MLA_Q_RANK, MLA_KV_RANK, MLA_ROPE,
             DIFF_W, DIFF_W, DIFF_W,
             FOX_W, FOX_W, FOX_W, FOX_HEADS)
IN_COLS = MLA_Q_RANK + MLA_KV_RANK + MLA_ROPE + 3 * DIFF_W + 3 * FOX_W + FOX_HEADS

N_EXPERTS = 16
N_GROUPS = 4
GROUP_SIZE = N_EXPERTS // N_GROUPS
TOP_K = 2
D_EXPERT = 512

DEEPNORM_ALPHA = (2 * DEPTH) ** 0.25
DEEPNORM_BETA = (8 * DEPTH) ** -0.25

LN_EPS = 1e-5
RMS_EPS = 1e-6
NEG_INF = -1e30
F32 = jnp.float32

kernel_name = "hybrid_mla_diff_fox_grouped_moe_deepnorm"


def layer_norm(x, g, b):
    xf = x.astype(F32)
    mu = jnp.mean(xf, axis=-1, keepdims=True)
    var = jnp.mean(jnp.square(xf - mu), axis=-1, keepdims=True)
    return ((xf - mu) * lax.rsqrt(var + LN_EPS) * g.astype(F32) + b.astype(F32)).astype(x.dtype)


def rms_norm(x, g):
    xf = x.astype(F32)
    return (xf * lax.rsqrt(jnp.mean(xf * xf, axis=-1, keepdims=True) + RMS_EPS) * g.astype(F32)).astype(x.dtype)


def rope_tables(positions, rot_dim):
    inv_freq = ROPE_THETA ** (-jnp.arange(0, rot_dim, 2, dtype=F32) / rot_dim)
    ang = positions.astype(F32)[..., None] * inv_freq
    return jnp.cos(ang), jnp.sin(ang)


def apply_rope(x, cos, sin):
    xf = x.astype(F32)
    x1, x2 = jnp.split(xf, 2, axis=-1)
    c = cos[:, :, None, :]
    s = sin[:, :, None, :]
    return jnp.concatenate([x1 * c - x2 * s, x2 * c + x1 * s], axis=-1).astype(x.dtype)


def partial_rope(x, cos, sin, rot_dim):
    return jnp.concatenate([apply_rope(x[..., :rot_dim], cos, sin), x[..., rot_dim:]], axis=-1)


def q_block(a, i, axis=1):
    return lax.dynamic_slice_in_dim(a, i * Q_BLOCK, Q_BLOCK, axis)


def chunk_causal_mask(i, seq):
    q_idx = i * Q_BLOCK + jnp.arange(Q_BLOCK)
    k_idx = jnp.arange(seq)
    return (k_idx[None, :] // CHUNK) <= (q_idx[:, None] // CHUNK)


def frame_causal_mask(i, seq):
    q_idx = i * Q_BLOCK + jnp.arange(Q_BLOCK)
    k_idx = jnp.arange(seq)
    return k_idx[None, :] <= q_idx[:, None]


def sweep_query_blocks(block_fn, seq):
    out = lax.map(block_fn, jnp.arange(seq // Q_BLOCK))
    out = jnp.moveaxis(out, 0, 1)
    return out.reshape(out.shape[0], seq, out.shape[3], out.shape[4])


def mla_attention(c_q, c_kv, k_rope_raw, q_norm_g, kv_norm_g, w_uq, w_ukv, cos, sin):
    b, seq, _ = c_q.shape
    q = (rms_norm(c_q, q_norm_g) @ w_uq).reshape(b, seq, MLA_HEADS, MLA_NOPE + MLA_ROPE)
    q_nope = q[..., :MLA_NOPE]
    q_rope = apply_rope(q[..., MLA_NOPE:], cos, sin)
    kv = (rms_norm(c_kv, kv_norm_g) @ w_ukv).reshape(b, seq, MLA_HEADS, MLA_NOPE + MLA_V)
    k_nope = kv[..., :MLA_NOPE]
    v = kv[..., MLA_NOPE:]
    k_rope = apply_rope(k_rope_raw[:, :, None, :], cos, sin)[:, :, 0, :]
    scale = (MLA_NOPE + MLA_ROPE) ** -0.5

    def block(i):
        sc = (jnp.einsum('bqhd,bkhd->bhqk', q_block(q_nope, i), k_nope, preferred_element_type=F32)
              + jnp.einsum('bqhr,bkr->bhqk', q_block(q_rope, i), k_rope, preferred_element_type=F32)) * scale
        sc = jnp.where(chunk_causal_mask(i, seq), sc, NEG_INF)
        p = jax.nn.softmax(sc, axis=-1).astype(v.dtype)
        return jnp.einsum('bhqk,bkhd->bqhd', p, v)

    return sweep_query_blocks(block, seq)


def diff_attention(q, k, v, lam_q1, lam_k1, lam_q2, lam_k2, subln_g, lam_init, cos, sin):
    b, seq, _ = q.shape
    q = partial_rope(q.reshape(b, seq, 2 * DIFF_HEADS, DIFF_QK), cos, sin, DIFF_ROT)
    k = partial_rope(k.reshape(b, seq, 2 * DIFF_HEADS, DIFF_QK), cos, sin, DIFF_ROT)
    q1, q2 = q[:, :, 0::2], q[:, :, 1::2]
    k1, k2 = k[:, :, 0::2], k[:, :, 1::2]
    v = v.reshape(b, seq, DIFF_HEADS, DIFF_V)
    lam = (jnp.exp(jnp.sum(lam_q1.astype(F32) * lam_k1.astype(F32)))
           - jnp.exp(jnp.sum(lam_q2.astype(F32) * lam_k2.astype(F32))) + lam_init)
    scale = DIFF_QK ** -0.5

    def block(i):
        mask = chunk_causal_mask(i, seq)
        s1 = jnp.einsum('bqhd,bkhd->bhqk', q_block(q1, i), k1, preferred_element_type=F32) * scale
        s2 = jnp.einsum('bqhd,bkhd->bhqk', q_block(q2, i), k2, preferred_element_type=F32) * scale
        p = (jax.nn.softmax(jnp.where(mask, s1, NEG_INF), axis=-1)
             - lam * jax.nn.softmax(jnp.where(mask, s2, NEG_INF), axis=-1)).astype(v.dtype)
        return jnp.einsum('bhqk,bkhd->bqhd', p, v)

    o = sweep_query_blocks(block, seq)
    return rms_norm(o, subln_g) * (1.0 - lam_init)


def forgetting_attention(q, k, v, f_logit, f_bias):
    b, seq, _ = q.shape
    q = q.reshape(b, seq, FOX_HEADS, HEAD_DIM)
    k = k.reshape(b, seq, FOX_HEADS, HEAD_DIM)
    v = v.reshape(b, seq, FOX_HEADS, HEAD_DIM)
    log_f = jax.nn.log_sigmoid(f_logit.astype(F32) + f_bias.astype(F32))
    cum = jnp.cumsum(log_f, axis=1).transpose(0, 2, 1)
    scale = HEAD_DIM ** -0.5

    def block(i):
        sc = jnp.einsum('bqhd,bkhd->bhqk', q_block(q, i), k, preferred_element_type=F32) * scale
        sc = sc + q_block(cum, i, axis=2)[..., :, None] - cum[..., None, :]
        sc = jnp.where(frame_causal_mask(i, seq), sc, NEG_INF)
        p = jax.nn.softmax(sc, axis=-1).astype(v.dtype)
        return jnp.einsum('bhqk,bkhd->bqhd', p, v)

    return sweep_query_blocks(block, seq)


def mixer_sublayer(h, cos_m, sin_m, cos_d, sin_d, w_in, mla_q_norm_g, mla_kv_norm_g, mla_w_uq, mla_w_ukv,
                   diff_lam_q1, diff_lam_k1, diff_lam_q2, diff_lam_k2, diff_subln_g, lam_init, fox_f_bias, w_out):
    b, seq, _ = h.shape
    proj = h @ w_in
    parts = []
    start = 0
    for width in IN_SPLITS:
        parts.append(proj[..., start:start + width])
        start += width
    c_q, c_kv, k_rope, dq, dk, dv, fq, fk, fv, ff = parts
    o_mla = mla_attention(c_q, c_kv, k_rope, mla_q_norm_g, mla_kv_norm_g, mla_w_uq, mla_w_ukv, cos_m, sin_m)
    o_diff = diff_attention(dq, dk, dv, diff_lam_q1, diff_lam_k1, diff_lam_q2, diff_lam_k2,
                            diff_subln_g, lam_init, cos_d, sin_d)
    o_fox = forgetting_attention(fq, fk, fv, ff, fox_f_bias)
    o = jnp.concatenate([o_mla.reshape(b, seq, -1), o_diff.reshape(b, seq, -1),
                         o_fox.reshape(b, seq, -1)], axis=-1)
    return o @ w_out


def grouped_moe(h, router_w, router_bias, w_gate, w_up, w_down):
    n = h.shape[0]
    scores = jax.nn.sigmoid(jnp.einsum('nd,de->ne', h, router_w, preferred_element_type=F32))
    biased = (scores + router_bias.astype(F32)).reshape(n, N_GROUPS, GROUP_SIZE)
    group_score = jnp.sum(lax.top_k(biased, TOP_K)[0], axis=-1)
    group_sel = jnp.argmax(group_score, axis=-1)
    in_group = group_sel[:, None] == jnp.arange(N_GROUPS)[None, :]
    cand = jnp.where(in_group[:, :, None], biased, NEG_INF).reshape(n, N_EXPERTS)
    _, idx = lax.top_k(cand, TOP_K)
    sel = jnp.take_along_axis(scores, idx, axis=-1)
    gates = sel / jnp.sum(sel, axis=-1, keepdims=True)
    dense_gates = jnp.einsum('nk,nke->ne', gates, jax.nn.one_hot(idx, N_EXPERTS, dtype=F32)).astype(h.dtype)
    y = jnp.zeros_like(h)
    for e in range(N_EXPERTS):
        a = jax.nn.silu(h @ w_gate[e]) * (h @ w_up[e])
        y = y + dense_gates[:, e:e + 1] * (a @ w_down[e])
    return y


def setup_inputs(seed: int = 0) -> dict:
    key = jax.random.key(seed)
    ks = jax.random.split(key, 24)

    def nrm(k, shape, scale):
        return jax.random.normal(k, shape, F32) * scale

    def gain(k, shape):
        return 1.0 + nrm(k, shape, 0.02)

    x = nrm(ks[0], (BATCH, SEQ, D_MODEL), 1.0)
    offsets = jax.random.randint(ks[1], (BATCH, 1), 0, 64, dtype=jnp.int32) * CHUNK
    positions = offsets + jnp.arange(SEQ, dtype=jnp.int32)[None, :]
    return {
        "x": x,
        "positions": positions,
        "ln_in_g": gain(ks[2], (D_MODEL,)),
        "ln_in_b": nrm(ks[3], (D_MODEL,), 0.02),
        "w_in": nrm(ks[4], (DEPTH, D_MODEL, IN_COLS), D_MODEL ** -0.5),
        "mla_q_norm_g": gain(ks[5], (DEPTH, MLA_Q_RANK)),
        "mla_kv_norm_g": gain(ks[6], (DEPTH, MLA_KV_RANK)),
        "mla_w_uq": nrm(ks[7], (DEPTH, MLA_Q_RANK, MLA_HEADS * (MLA_NOPE + MLA_ROPE)), MLA_Q_RANK ** -0.5),
        "mla_w_ukv": nrm(ks[8], (DEPTH, MLA_KV_RANK, MLA_HEADS * (MLA_NOPE + MLA_V)), MLA_KV_RANK ** -0.5),
        "diff_lam_q1": nrm(ks[9], (DEPTH, DIFF_QK), 0.1),
        "diff_lam_k1": nrm(ks[10], (DEPTH, DIFF_QK), 0.1),
        "diff_lam_q2": nrm(ks[11], (DEPTH, DIFF_QK), 0.1),
        "diff_lam_k2": nrm(ks[12], (DEPTH, DIFF_QK), 0.1),
        "diff_subln_g": gain(ks[13], (DEPTH, DIFF_V)),
        "fox_f_bias": jax.random.uniform(ks[14], (DEPTH, FOX_HEADS), F32, 1.0, 4.0),
        "w_out": nrm(ks[15], (DEPTH, D_MIX, D_MODEL), D_MIX ** -0.5 * DEEPNORM_BETA),
        "ln1_g": gain(ks[16], (DEPTH, D_MODEL)),
        "ln1_b": nrm(ks[17], (DEPTH, D_MODEL), 0.02),
        "router_w": nrm(ks[18], (D_MODEL, N_EXPERTS), D_MODEL ** -0.5),
        "router_bias": nrm(ks[19], (N_EXPERTS,), 0.01),
        "exp_w_gate": nrm(ks[20], (DEPTH, N_EXPERTS, D_MODEL, D_EXPERT), D_MODEL ** -0.5),
        "exp_w_up": nrm(ks[21], (DEPTH, N_EXPERTS, D_MODEL, D_EXPERT), D_MODEL ** -0.5),
        "exp_w_down": nrm(ks[22], (DEPTH, N_EXPERTS, D_EXPERT, D_MODEL), D_EXPERT ** -0.5 * DEEPNORM_BETA),
        "ln2_g": gain(ks[23], (DEPTH, D_MODEL)),
        "ln2_b": nrm(jax.random.fold_in(ks[23], 1), (DEPTH, D_MODEL), 0.02),
    }


def reference(x, positions, ln_in_g, ln_in_b, w_in, mla_q_norm_g, mla_kv_norm_g, mla_w_uq, mla_w_ukv,
              diff_lam_q1, diff_lam_k1, diff_lam_q2, diff_lam_k2, diff_subln_g, fox_f_bias, w_out,
              ln1_g, ln1_b, router_w, router_bias, exp_w_gate, exp_w_up, exp_w_down, ln2_g, ln2_b):
    b, seq, d = x.shape
    cos_m, sin_m = rope_tables(positions, MLA_ROPE)
    cos_d, sin_d = rope_tables(positions, DIFF_ROT)
    h = layer_norm(x, ln_in_g, ln_in_b)
    for l in range(DEPTH):
        lam_init = 0.8 - 0.6 * math.exp(-0.3 * l)
        mix = mixer_sublayer(h, cos_m, sin_m, cos_d, sin_d, w_in[l], mla_q_norm_g[l], mla_kv_norm_g[l],
                             mla_w_uq[l], mla_w_ukv[l], diff_lam_q1[l], diff_lam_k1[l], diff_lam_q2[l],
                             diff_lam_k2[l], diff_subln_g[l], lam_init, fox_f_bias[l], w_out[l])
        h = layer_norm(DEEPNORM_ALPHA * h + mix, ln1_g[l], ln1_b[l])
        ffn = grouped_moe(h.reshape(b * seq, d), router_w, router_bias,
                          exp_w_gate[l], exp_w_up[l], exp_w_down[l]).reshape(b, seq, d)
        h = layer_norm(DEEPNORM_ALPHA * h + ffn, ln2_g[l], ln2_b[l])
    return h
```

```python
import math
import os
from contextlib import ExitStack

import numpy as np
import ml_dtypes
import concourse.bass as bass
import concourse.mybir as mybir
from concourse.bass_utils import run_bass_kernel_spmd

F32 = mybir.dt.float32
BF16 = mybir.dt.bfloat16
I32 = mybir.dt.int32
AF = mybir.ActivationFunctionType
ALU = mybir.AluOpType
AX = mybir.AxisListType
NPBF = ml_dtypes.bfloat16

D = 1024
IN_COLS = 2212
BLK = 512
LN_EPS = 1e-5
RMS_EPS = 1e-6
ALPHA = 4.0 ** 0.25
PI = math.pi


class Op:
    __slots__ = ("eng", "fn", "deps", "needs_inc", "sem", "val", "dma", "semkey")

    def __init__(self, eng, fn, dma, semkey):
        self.eng = eng
        self.fn = fn
        self.deps = []
        self.needs_inc = False
        self.sem = None
        self.val = 0
        self.dma = dma
        self.semkey = semkey


class Sched:
    ENGS = ("pe", "act", "dve", "pool", "sp")

    def __init__(self):
        self.ops = {e: [] for e in self.ENGS}
        self.last_writer = {}
        self.readers = {}
        self.dma_keys = {}
        self.finals = {}

    def add(self, eng, fn, reads=(), writes=(), dma=False, semkey=None, extra=(), final=False):
        op = Op(eng, fn, dma, semkey)
        deps = {}
        for r in reads:
            w = self.last_writer.get(r)
            if w is not None:
                deps[id(w)] = w
            if r.startswith("ps"):
                rd = self.readers.get(r)
                if rd:
                    for x in rd.values():
                        if x.eng != eng:
                            deps[id(x)] = x
        for r in writes:
            w = self.last_writer.get(r)
            if w is not None:
                deps[id(w)] = w
            rd = self.readers.get(r)
            if rd:
                for x in rd.values():
                    deps[id(x)] = x
        for x in extra:
            deps[id(x)] = x
        for d in deps.values():
            if (not d.dma) and (not dma) and d.eng == eng and (eng == "pe" or eng in os.environ.get("NOSAME", "").split(",")):
                continue
            op.deps.append(d)
            if not d.dma:
                d.needs_inc = True
        for r in reads:
            rd = self.readers.setdefault(r, {})
            if dma:
                rd[("dma", id(op))] = op
            else:
                rd[eng] = op
        for r in writes:
            self.last_writer[r] = op
            self.readers[r] = {}
        if dma:
            c = self.dma_keys.get(semkey, 0) + 1
            self.dma_keys[semkey] = c
            op.val = 16 * c
        self.ops[eng].append(op)
        if final:
            self.finals[semkey if dma else id(op)] = op
        return op

    def pe(self, fn, reads=(), writes=(), **k):
        return self.add("pe", fn, reads, writes, **k)

    def act(self, fn, reads=(), writes=(), **k):
        return self.add("act", fn, reads, writes, **k)

    def dve(self, fn, reads=(), writes=(), **k):
        return self.add("dve", fn, reads, writes, **k)

    def pool(self, fn, reads=(), writes=(), **k):
        return self.add("pool", fn, reads, writes, **k)

    def dma(self, fn, reads=(), writes=(), semkey=None, eng="sp", **k):
        return self.add(eng, fn, reads, writes, dma=True, semkey=semkey, **k)

    def emit(self, nc, stack):
        esem = {e: stack.enter_context(nc.semaphore("s_" + e)) for e in self.ENGS}
        dsem = {k: stack.enter_context(nc.semaphore("d_" + str(k))) for k in self.dma_keys}
        for e in self.ENGS:
            c = 0
            for op in self.ops[e]:
                if op.dma:
                    op.sem = dsem[op.semkey]
                else:
                    if op.needs_inc:
                        c += 1
                        op.val = c
                    op.sem = esem[e]
        finals = list(self.finals.values())
        block = stack.enter_context(nc.Block())

        def run(engname, eng, with_final=False):
            waited = {}

            def wait(d):
                key = id(d.sem)
                if waited.get(key, 0) >= d.val:
                    return
                eng.wait_ge(d.sem, d.val)
                waited[key] = d.val

            for op in self.ops[engname]:
                for d in op.deps:
                    wait(d)
                ins = op.fn(eng)
                if op.dma:
                    ins.then_inc(op.sem, 16)
                elif op.needs_inc:
                    ins.then_inc(op.sem, 1)
            if with_final:
                for d in finals:
                    wait(d)

        @block.sync
        def _(eng):
            run("sp", eng, True)

        @block.tensor
        def _(eng):
            run("pe", eng)

        @block.scalar
        def _(eng):
            run("act", eng)

        @block.vector
        def _(eng):
            run("dve", eng)

        @block.gpsimd
        def _(eng):
            run("pool", eng)


class Ctx:
    def __init__(self):
        self.nc = bass.Bass("TRN2", target_bir_lowering=False)
        self.S = Sched()
        self.st = ExitStack()
        self.psi = 0
        self.nps = 0
        self.ps = []
        self.pools = {}
        self.evi = 0

    def din(self, name, shape, dt=F32):
        return self.nc.dram_tensor(name, list(shape), dt, kind="ExternalInput").ap()

    def dout(self, name, shape, dt=F32):
        return self.nc.dram_tensor(name, list(shape), dt, kind="ExternalOutput").ap()

    def sb(self, name, shape, dt=F32):
        return self.st.enter_context(self.nc.sbuf_tensor(name, list(shape), dt))

    def psum_banks(self, n):
        for i in range(n):
            t = self.st.enter_context(self.nc.psum_tensor("psb%d" % i, [128, 512], F32))
            self.ps.append(t)

    def bank(self, pool=None):
        if pool is None:
            i = self.psi % len(self.ps)
            self.psi += 1
        else:
            lo, hi = pool
            k = self.pools.get(pool, 0)
            self.pools[pool] = k + 1
            i = lo + k % (hi - lo)
        return self.ps[i], "psb%d" % i

    def finish(self):
        self.S.emit(self.nc, self.st)
        self.st.close()
        return self.nc


def build_A(T, first):
    c = Ctx()
    S = c.S
    NB = T // BLK
    hin = c.din("hin", [T, D])
    posr = c.din("posr", [128, T], I32)
    invf = c.din("invf", [128, 3])
    ident_d = c.din("ident", [128, 128])
    w_in_d = c.din("w_in", [128, 8, IN_COLS])
    wuq_d = c.din("wuq", [128, 3, 768])
    wuk_d = c.din("wuk", [128, 2, 512])
    wuv_d = c.din("wuv", [128, 2, 512])
    gq_d = c.din("gq", [128, 3])
    gkv_d = c.din("gkv", [128, 2])
    fb_d = c.din("fbias", [128, 4])
    if first:
        lng_d = c.din("lng", [128, D])
        lnb_d = c.din("lnb", [128, D])
        hres = c.dout("hres", [T, D])
    qm_o = c.dout("qm", [8, 96, T], BF16)
    qd_o = c.dout("qd", [2, 128, T], BF16)
    qf_o = c.dout("qf", [2, 128, T], BF16)
    kmn_o = c.dout("kmn", [4, 128, T], BF16)
    kr_o = c.dout("kr", [32, T], BF16)
    kd_o = c.dout("kd", [2, 128, T], BF16)
    kf_o = c.dout("kf", [2, 128, T], BF16)
    vm_o = c.dout("vm", [T, 512], BF16)
    vdf_o = c.dout("vdf", [T, 512], BF16)
    lf_o = c.dout("lf", [T, 4])

    w_in = c.sb("w_in_sb", [128, 8, 2240], BF16)
    w_rot = c.sb("w_rot_sb", [128, 8, 544], BF16)
    wuq = c.sb("wuq_sb", [128, 3, 768], BF16)
    wuq_rot = c.sb("wuqr_sb", [128, 3, 768], BF16)
    wuk = c.sb("wuk_sb", [128, 2, 512], BF16)
    wuv = c.sb("wuv_sb", [128, 2, 512], BF16)
    wst2 = [c.sb("wstage%d" % i, [128, 2304], F32) for i in range(2)]
    wst = wst2[0]
    gq = c.sb("gq_sb", [128, 3])
    gkv = c.sb("gkv_sb", [128, 2])
    fb = c.sb("fb_sb", [128, 4])
    invf_s = c.sb("invf_sb", [128, 3])
    posi = c.sb("posi", [128, BLK], I32)
    posf = c.sb("posf", [128, BLK])
    identf = c.sb("identf", [128, 128])
    ident = c.sb("identb", [128, 128], BF16)
    ones = c.sb("onesb", [128, 128], BF16)
    if first:
        lng = c.sb("lng_sb", [128, D])
        lnb = c.sb("lnb_sb", [128, D])
    c.psum_banks(6)
    pst = [c.st.enter_context(c.nc.psum_tensor("pst%d" % i, [128, 1024], BF16)) for i in range(2)]

    def load(dst, src, key):
        return S.dma(lambda e: e.dma_start(out=dst, in_=src), writes=[key], semkey=key)

    load(gq[:], gq_d, "gq")
    load(gkv[:], gkv_d, "gkv")
    load(fb[:], fb_d, "fb")
    load(invf_s[:], invf, "invf")
    load(identf[:], ident_d, "identf")
    if os.environ.get("E3"):
        load(posi[:], posr[:, 0:BLK], "posi")
    if first:
        load(lng[:], lng_d, "lng")
        load(lnb[:], lnb_d, "lnb")
    S.dve(lambda e: e.tensor_copy(ident[:], identf[:]), reads=["identf"], writes=["ident"])
    S.dve(lambda e: e.memset(ones[:], 1.0), writes=["ones"])

    for k in range(8):
        ws = wst2[k % 2]
        wk_ = "wst%d" % (k % 2)
        load(ws[:, 0:IN_COLS], w_in_d[:, k, :], wk_)
        eng = S.dve if k % 2 == 0 else S.pool
        eng(lambda e, k=k, ws=ws: e.tensor_copy(w_in[:, k, 0:IN_COLS], ws[:, 0:IN_COLS]), reads=[wk_], writes=["w_in%d" % k])
    S.pool(lambda e: e.memset(w_rot[:], 0.0), writes=["w_rot"])
    rr = ["w_in%d" % k for k in range(8)]
    S.dve(lambda e: e.tensor_scalar(w_rot[:, :, 0:16], w_in[:, :, 656:672], -1.0, None, ALU.mult),
          reads=rr + ["w_rot"], writes=["w_rot"])
    S.dve(lambda e: e.tensor_copy(w_rot[:, :, 16:32], w_in[:, :, 640:656]), reads=rr + ["w_rot"], writes=["w_rot"])
    for j, c0 in enumerate((672, 928)):
        src = w_in[:, :, c0:c0 + 256].rearrange("p k (s d) -> p k s d", d=32)
        dst = w_rot[:, :, 32 + 256 * j:32 + 256 * (j + 1)].rearrange("p k (s d) -> p k s d", d=32)
        for k in range(8):
            S.dve(lambda e, k=k, src=src, dst=dst: e.tensor_scalar(dst[:, k, :, 0:4], src[:, k, :, 4:8], -1.0, None, ALU.mult),
                  reads=rr + ["w_rot"], writes=["w_rot"])
            S.dve(lambda e, k=k, src=src, dst=dst: e.tensor_copy(dst[:, k, :, 4:8], src[:, k, :, 0:4]),
                  reads=rr + ["w_rot"], writes=["w_rot"])
    wq_v = wst[:, 0:3 * 768].rearrange("p (k c) -> p k c", k=3)
    load(wq_v, wuq_d, "wst0")
    for k in range(3):
        S.dve(lambda e, k=k: e.tensor_scalar(wuq[:, k, :], wq_v[:, k, :], gq[:, k:k + 1], None, ALU.mult),
              reads=["wst0", "gq"], writes=["wuq"])
    S.pool(lambda e: e.memset(wuq_rot[:], 0.0), writes=["wuqr"])
    wq4 = wuq[:].rearrange("p k (h d) -> p k h d", d=96)
    wr4 = wuq_rot[:].rearrange("p k (h d) -> p k h d", d=96)
    for k in range(3):
        S.dve(lambda e, k=k: e.tensor_scalar(wr4[:, k, :, 64:80], wq4[:, k, :, 80:96], -1.0, None, ALU.mult),
              reads=["wuq", "wuqr"], writes=["wuqr"])
        S.dve(lambda e, k=k: e.tensor_copy(wr4[:, k, :, 80:96], wq4[:, k, :, 64:80]),
              reads=["wuq", "wuqr"], writes=["wuqr"])
    wk_v = wst[:, 0:1024].rearrange("p (k c) -> p k c", k=2)
    load(wk_v, wuk_d, "wst0")
    for k in range(2):
        S.dve(lambda e, k=k: e.tensor_scalar(wuk[:, k, :], wk_v[:, k, :], gkv[:, k:k + 1], None, ALU.mult),
              reads=["wst0", "gkv"], writes=["wuk"])
    load(wk_v, wuv_d, "wst0")
    for k in range(2):
        S.dve(lambda e, k=k: e.tensor_scalar(wuv[:, k, :], wk_v[:, k, :], gkv[:, k:k + 1], None, ALU.mult),
              reads=["wst0", "gkv"], writes=["wuv"])
    WIN = rr + ["w_rot"]

    xt = [c.sb("xt%d" % i, [128, D]) for i in range(2)]
    hb = [c.sb("hb%d" % i, [128, D], BF16) for i in range(4)]
    hT = c.sb("hT", [128, 8, BLK], BF16)
    junk = c.sb("junk", [128, D], BF16)
    stat = c.sb("stat", [128, 8])
    tabs = c.sb("tabs", [128, 6, BLK])
    targ = c.sb("targ", [128, BLK])
    targ2 = c.sb("targ2", [128, BLK])
    targi = c.sb("targi", [128, BLK], I32)
    cq_raw = c.sb("cq_raw", [128, 5, BLK], BF16)
    sq = c.sb("sq", [128, 5, BLK], BF16)
    rstd = c.sb("rstd", [128, 2, BLK])
    cn = c.sb("cn", [128, 5, BLK], BF16)
    t1 = [c.sb("t1_%d" % i, [128, BLK]) for i in range(2)]
    t2 = [c.sb("t2_%d" % i, [128, BLK]) for i in range(2)]
    ob = [c.sb("ob%d" % i, [128, BLK], BF16) for i in range(4)]
    vb = [c.sb("vb%d" % i, [128, 512], BF16) for i in range(2)]
    lft = [c.sb("lft%d" % i, [128, 4]) for i in range(2)]
    cnt = {"ob": 0, "t": 0, "vb": 0, "ev": 0}

    def store(dst, src, rkey, final=True):
        return S.dma(lambda e: e.dma_start(out=dst, in_=src), reads=[rkey], semkey="st_" + rkey, final=final)

    STOP = float(os.environ.get("STOPA", "99"))
    if STOP < 1:
        return c.finish()
    for b in range(NB):
        t0 = b * BLK
        for tt in range(4):
            r0 = t0 + tt * 128
            x = xt[tt % 2]
            xk = "xt%d" % (tt % 2)
            load(x[:], hin[r0:r0 + 128, :], xk)
            if first:
                S.act(lambda e, x=x: e.activation(out=junk[:], in_=x[:], func=AF.Identity, accum_out=stat[:, 0:1]),
                      reads=[xk], writes=["junk", "stat"])
                S.act(lambda e, x=x: e.activation(out=junk[:], in_=x[:], func=AF.Square, accum_out=stat[:, 1:2]),
                      reads=[xk], writes=["junk", "stat"])
                S.dve(lambda e: e.tensor_scalar(stat[:, 2:4], stat[:, 0:2], 1.0 / D, None, ALU.mult),
                      reads=["stat"], writes=["stat"])
                S.dve(lambda e: e.tensor_tensor(stat[:, 4:5], stat[:, 2:3], stat[:, 2:3], ALU.mult),
                      reads=["stat"], writes=["stat"])
                S.dve(lambda e: e.tensor_tensor(stat[:, 5:6], stat[:, 3:4], stat[:, 4:5], ALU.subtract),
                      reads=["stat"], writes=["stat"])
                S.dve(lambda e: e.tensor_scalar(stat[:, 6:7], stat[:, 5:6], LN_EPS, None, ALU.add),
                      reads=["stat"], writes=["stat"])
                S.act(lambda e: e.activation(out=stat[:, 6:7], in_=stat[:, 6:7], func=AF.Ln), reads=["stat"], writes=["stat"])
                S.act(lambda e: e.activation(out=stat[:, 6:7], in_=stat[:, 6:7], func=AF.Exp, scale=-0.5), reads=["stat"], writes=["stat"])
                S.dve(lambda e, x=x: e.tensor_scalar(x[:], x[:], stat[:, 2:3], stat[:, 6:7], ALU.subtract, ALU.mult),
                      reads=[xk, "stat"], writes=[xk])
                S.dve(lambda e, x=x: e.tensor_tensor(x[:], x[:], lng[:], ALU.mult), reads=[xk, "lng"], writes=[xk])
                S.dve(lambda e, x=x: e.tensor_tensor(x[:], x[:], lnb[:], ALU.add), reads=[xk, "lnb"], writes=[xk])
                store(hres[r0:r0 + 128, :], x[:], xk)
            S.act(lambda e, x=x, tt=tt: e.copy(hb[tt][:], x[:]), reads=[xk], writes=["hb%d" % tt])
        if STOP < 2:
            return c.finish()
        for k in range(8):
            pt = pst[k % 2]
            pk = "pst%d" % (k % 2)
            for tt in range(4):
                S.pe(lambda e, pt=pt, k=k, tt=tt: e.transpose(pt[:, tt * 128:(tt + 1) * 128], hb[tt][:, k * 128:(k + 1) * 128], ident[:]),
                     reads=["hb%d" % tt, "ident"], writes=[pk])
            ev = S.act if k % 2 == 0 else S.dve
            if k % 2 == 0:
                S.act(lambda e, pt=pt, k=k: e.copy(hT[:, k, :], pt[:, 0:BLK]), reads=[pk], writes=["hT%d" % k])
            else:
                S.dve(lambda e, pt=pt, k=k: e.tensor_copy(hT[:, k, :], pt[:, 0:BLK]), reads=[pk], writes=["hT%d" % k])
        HT = ["hT%d" % k for k in range(8)]
        if STOP < 3:
            return c.finish()
        if os.environ.get("E1"):
            load(posi[:], posr, "posi")
        elif os.environ.get("E3"):
            pass
        else:
            load(posi[:], posr[:, t0:t0 + BLK], "posi")
        if os.environ.get("NOCOPY") is None:
            S.dve(lambda e: e.tensor_copy(posf[:], posi[:]), reads=["posi"], writes=["posf"])
        for j in range(int(os.environ.get("NJ", "3"))):
            for cs, off in ((0, 0.25), (1, 0.0)):
                S.dve(lambda e, j=j, off=off: e.tensor_scalar(targ[:], posf[:], invf_s[:, j:j + 1], off, ALU.mult, ALU.add),
                      reads=["posf", "invf"], writes=["targ"])
                S.dve(lambda e: e.tensor_copy(targi[:], targ[:]), reads=["targ"], writes=["targi"])
                S.dve(lambda e: e.tensor_copy(targ2[:], targi[:]), reads=["targi"], writes=["targ2"])
                S.dve(lambda e: e.tensor_tensor(targ[:], targ[:], targ2[:], ALU.subtract), reads=["targ", "targ2"], writes=["targ"])
                S.dve(lambda e: e.tensor_scalar(targ2[:], targ[:], 0.0, None, ALU.is_lt), reads=["targ"], writes=["targ2"])
                S.dve(lambda e: e.tensor_tensor(targ[:], targ[:], targ2[:], ALU.add), reads=["targ", "targ2"], writes=["targ"])
                S.dve(lambda e: e.tensor_scalar(targ[:], targ[:], 2 * PI, -PI, ALU.mult, ALU.add), reads=["targ"], writes=["targ"])
                S.act(lambda e, j=j, cs=cs: e.activation(out=tabs[:, 2 * j + cs, :], in_=targ[:], func=AF.Sin, scale=-1.0),
                      reads=["targ"], writes=["tabs"])

        def group(M, parts, wreads):
            pb, pk = c.bank()
            n = len(parts)
            for i, (l, r) in enumerate(parts):
                S.pe(lambda e, l=l, r=r, i=i, pb=pb: e.matmul(pb[0:M, 0:r.shape[-1]], lhsT=l, rhs=r, start=(i == 0), stop=(i == n - 1)),
                     reads=wreads, writes=[pk])
            return pb, pk

        def inproj(c0, M, rot=False):
            w = w_rot if rot else w_in
            return group(M, [(w[:, k, c0:c0 + M], hT[:, k, :]) for k in range(8)], HT + WIN)

        def rope_out(pq, kq, pr, kr_, M, tj, dst):
            i = cnt["t"] % 2
            cnt["t"] += 1
            a, b_ = t1[i], t2[i]
            S.dve(lambda e: e.tensor_tensor(a[0:M, :], pq[0:M, :], tabs[0:M, 2 * tj, :], ALU.mult),
                  reads=[kq, "tabs"], writes=["t1_%d" % i])
            S.dve(lambda e: e.tensor_tensor(b_[0:M, :], pr[0:M, :], tabs[0:M, 2 * tj + 1, :], ALU.mult),
                  reads=[kr_, "tabs"], writes=["t2_%d" % i])
            o = cnt["ob"] % 4
            cnt["ob"] += 1
            S.pool(lambda e: e.tensor_tensor(ob[o][0:M, :], a[0:M, :], b_[0:M, :], ALU.add),
                   reads=["t1_%d" % i, "t2_%d" % i], writes=["ob%d" % o])
            store(dst, ob[o][0:M, :], "ob%d" % o)

        def plain_out(pq, kq, M, dst):
            o = cnt["ob"] % 4
            cnt["ob"] += 1
            cnt["ev"] += 1
            if cnt["ev"] % 2:
                S.act(lambda e: e.copy(ob[o][0:M, :], pq[0:M, :]), reads=[kq], writes=["ob%d" % o])
            else:
                S.dve(lambda e: e.tensor_copy(ob[o][0:M, :], pq[0:M, :]), reads=[kq], writes=["ob%d" % o])
            store(dst, ob[o][0:M, :], "ob%d" % o)

        if STOP < 4:
            return c.finish()
        for g in range(5):
            pb, pk = inproj(g * 128, 128)
            if STOP == 4.1 and g + 1 >= int(os.environ.get("NG", "1")):
                return c.finish()
            if STOP == 4.1:
                continue
            S.act(lambda e, pb=pb, g=g: e.activation(out=sq[:, g, :], in_=pb[:], func=AF.Square), reads=[pk], writes=["sq%d" % g])
            if STOP == 4.2 and g + 1 >= int(os.environ.get("NG", "1")):
                return c.finish()
            if STOP == 4.2:
                continue
            S.dve(lambda e, pb=pb, g=g: e.tensor_copy(cq_raw[:, g, :], pb[:]), reads=[pk], writes=["cqr%d" % g])
            if STOP == 4.3 and g + 1 >= int(os.environ.get("NG", "1")):
                return c.finish()
        if STOP == 4.4:
            return c.finish()
        for j, (g0, g1, n) in enumerate(((0, 3, 384), (3, 5, 256))):
            pb, pk = group(128, [(ones[:], sq[:, g, :]) for g in range(g0, g1)], ["ones"] + ["sq%d" % g for g in range(g0, g1)])
            S.dve(lambda e, pb=pb, j=j, n=n: e.tensor_scalar(rstd[:, j, :], pb[:], 1.0 / n, RMS_EPS, ALU.mult, ALU.add),
                  reads=[pk], writes=["rstd%d" % j])
            if STOP == 4.5:
                return c.finish()
            S.act(lambda e, j=j: e.activation(out=rstd[:, j, :], in_=rstd[:, j, :], func=AF.Ln),
                  reads=["rstd%d" % j], writes=["rstd%d" % j])
            S.act(lambda e, j=j: e.activation(out=rstd[:, j, :], in_=rstd[:, j, :], func=AF.Exp, scale=-0.5),
                  reads=["rstd%d" % j], writes=["rstd%d" % j])
            if STOP == 4.6:
                return c.finish()
            for g in range(g0, g1):
                S.dve(lambda e, g=g, j=j: e.tensor_tensor(cn[:, g, :], cq_raw[:, g, :], rstd[:, j, :], ALU.mult),
                      reads=["cqr%d" % g, "rstd%d" % j], writes=["cn%d" % g])
        CQ = ["cn0", "cn1", "cn2"]
        CKV = ["cn3", "cn4"]
        if STOP < 5:
            return c.finish()
        pq, kq = inproj(640, 32)
        pr, kr_ = inproj(0, 32, rot=True)
        rope_out(pq, kq, pr, kr_, 32, 2, kr_o[:, t0:t0 + BLK])
        for j, (c0, dst) in enumerate(((672, qd_o), (928, kd_o))):
            for g in range(2):
                pq, kq = inproj(c0 + g * 128, 128)
                pr, kr_ = inproj(32 + 256 * j + g * 128, 128, rot=True)
                rope_out(pq, kq, pr, kr_, 128, 1, dst[g, :, t0:t0 + BLK])
        if STOP < 6:
            return c.finish()
        for c0, dst in ((1440, qf_o), (1696, kf_o)):
            for g in range(2):
                pq, kq = inproj(c0 + g * 128, 128)
                plain_out(pq, kq, 128, dst[g, :, t0:t0 + BLK])
        if STOP < 7:
            return c.finish()
        for tt in range(4):
            r0 = t0 + tt * 128
            pb, pk = group(128, [(hT[:, k, tt * 128:(tt + 1) * 128], w_in[:, k, 1184:1440]) for k in range(8)], HT + WIN)
            pb2, pk2 = group(128, [(hT[:, k, tt * 128:(tt + 1) * 128], w_in[:, k, 1952:2208]) for k in range(8)], HT + WIN)
            v = cnt["vb"] % 2
            cnt["vb"] += 1
            S.act(lambda e, pb=pb, v=v: e.copy(vb[v][:, 0:256], pb[:, 0:256]), reads=[pk], writes=["vb%d" % v])
            S.dve(lambda e, pb2=pb2, v=v: e.tensor_copy(vb[v][:, 256:512], pb2[:, 0:256]), reads=[pk2, "vb%d" % v], writes=["vb%d" % v])
            store(vdf_o[r0:r0 + 128, :], vb[v][:], "vb%d" % v)
            pb, pk = group(128, [(hT[:, k, tt * 128:(tt + 1) * 128], w_in[:, k, 2208:2212]) for k in range(8)], HT + WIN)
            lt = lft[tt % 2]
            lk = "lft%d" % (tt % 2)
            S.dve(lambda e, pb=pb, lt=lt: e.tensor_tensor(lt[:], pb[:, 0:4], fb[:], ALU.add), reads=[pk, "fb"], writes=[lk])
            S.act(lambda e, lt=lt: e.activation(out=lt[:], in_=lt[:], func=AF.Exp, scale=-1.0), reads=[lk], writes=[lk])
            S.dve(lambda e, lt=lt: e.tensor_scalar(lt[:], lt[:], 1.0, None, ALU.add), reads=[lk], writes=[lk])
            S.act(lambda e, lt=lt: e.activation(out=lt[:], in_=lt[:], func=AF.Ln), reads=[lk], writes=[lk])
            S.dve(lambda e, lt=lt: e.tensor_scalar(lt[:], lt[:], -1.0, None, ALU.mult), reads=[lk], writes=[lk])
            store(lf_o[r0:r0 + 128, :], lt[:], lk)
        if STOP < 8:
            return c.finish()
        for h in range(8):
            pq, kq = group(96, [(wuq[:, k, h * 96:(h + 1) * 96], cn[:, k, :]) for k in range(3)], CQ + ["wuq"])
            pr, kr_ = group(96, [(wuq_rot[:, k, h * 96:(h + 1) * 96], cn[:, k, :]) for k in range(3)], CQ + ["wuqr"])
            rope_out(pq, kq, pr, kr_, 96, 0, qm_o[h, :, t0:t0 + BLK])
        for g in range(4):
            pq, kq = group(128, [(wuk[:, k, g * 128:(g + 1) * 128], cn[:, 3 + k, :]) for k in range(2)], CKV + ["wuk"])
            plain_out(pq, kq, 128, kmn_o[g, :, t0:t0 + BLK])
        for tt in range(4):
            r0 = t0 + tt * 128
            pb, pk = group(128, [(cn[:, 3 + k, tt * 128:(tt + 1) * 128], wuv[:, k, :]) for k in range(2)], CKV + ["wuv"])
            v = cnt["vb"] % 2
            cnt["vb"] += 1
            S.act(lambda e, pb=pb, v=v: e.copy(vb[v][:], pb[:]), reads=[pk], writes=["vb%d" % v])
            store(vm_o[r0:r0 + 128, :], vb[v][:], "vb%d" % v)
    return c.finish()


def kchunk(w, nk):
    return np.ascontiguousarray(w.reshape(nk, 128, -1).transpose(1, 0, 2))


def bcast_rows(v, n=128):
    return np.ascontiguousarray(np.broadcast_to(np.asarray(v)[None, :], (n, len(v))))


def const_invf():
    fm = (500000.0 ** (-np.arange(0, 32, 2, dtype=np.float32) / 32)).astype(np.float32)
    fd = (500000.0 ** (-np.arange(0, 8, 2, dtype=np.float32) / 8)).astype(np.float32)
    t = np.zeros((128, 3), np.float32)
    for p in range(128):
        if 64 <= p < 96:
            t[p, 0] = fm[(p - 64) % 16]
        if p % 32 < 8:
            t[p, 1] = fd[(p % 32) % 4]
        if p < 32:
            t[p, 2] = fm[p % 16]
    return (t / np.float32(2 * np.pi)).astype(np.float32)


def weights_A(inp, l):
    wukv = inp["mla_w_ukv"][l].reshape(256, 8, 128)
    return {
        "invf": const_invf(),
        "ident": np.eye(128, dtype=np.float32),
        "w_in": kchunk(inp["w_in"][l], 8),
        "wuq": kchunk(inp["mla_w_uq"][l], 3),
        "wuk": kchunk(np.ascontiguousarray(wukv[:, :, 0:64]).reshape(256, 512), 2),
        "wuv": kchunk(np.ascontiguousarray(wukv[:, :, 64:128]).reshape(256, 512), 2),
        "gq": np.ascontiguousarray(inp["mla_q_norm_g"][l].reshape(3, 128).T),
        "gkv": np.ascontiguousarray(inp["mla_kv_norm_g"][l].reshape(2, 128).T),
        "fbias": bcast_rows(inp["fox_f_bias"][l]),
    }


def build_B(S_, lam_init):
    c = Ctx()
    S = c.S
    NB = S_ // BLK
    NKT = S_ // 128
    qm_d = c.din("qm", [4, 96, S_], BF16)
    km_d = c.din("km", [4, 96, S_], BF16)
    vm_d = c.din("vm", [S_, 256], BF16)
    qd_d = c.din("qd", [4, 32, S_], BF16)
    kd_d = c.din("kd", [4, 32, S_], BF16)
    vd_d = c.din("vd", [S_, 128], BF16)
    qf_d = c.din("qf", [2, 64, S_], BF16)
    kf_d = c.din("kf", [2, 64, S_], BF16)
    vf_d = c.din("vf", [S_, 128], BF16)
    lf_d = c.din("lf", [S_, 2])
    lam_d = c.din("lamv", [128, 4, 32])
    subg_d = c.din("subg", [64, 1])
    utri_d = c.din("utri", [128, 128])
    ident_d = c.din("ident", [128, 128])
    oT = c.dout("oT", [512, S_], BF16)
    cum_scr = c.nc.dram_tensor("cum_scr", [2, S_], F32).ap()

    NS = 2
    qt = [c.sb("qt%d" % i, [96, S_], BF16) for i in range(NS)]
    kt_ = [c.sb("kt%d" % i, [96, S_], BF16) for i in range(NS)]
    vt = [c.sb("vt%d" % i, [128, NKT, 65], BF16) for i in range(NS)]
    pT = [c.sb("pT%d" % i, [128, BLK], BF16) for i in range(4)]
    onesf = c.sb("onesf", [128, 128])
    ones_b = c.sb("ones_b", [128, 128], BF16)
    utri = c.sb("utri_sb", [128, 128])
    lamv = c.sb("lamv_sb", [128, 4, 32])
    lamt = c.sb("lamt", [128, 8])
    subg = c.sb("subg_sb", [64, 1])
    rl = c.sb("rl", [128, 2, BLK])
    osb = [c.sb("osb%d" % i, [64, BLK]) for i in range(2)]
    dsb = c.sb("dsb", [64, BLK])
    dsq = c.sb("dsq", [64, BLK], BF16)
    rsd = c.sb("rsd", [64, BLK])
    oout = [c.sb("oout%d" % i, [64, BLK], BF16) for i in range(2)]
    lft = c.sb("lft", [128, NKT, 2])
    cumT = c.sb("cumT", [128, 2, NKT])
    carry = c.sb("carry", [128, 2])
    cb = c.sb("cb", [128, 2, NB])
    csel = c.sb("csel", [128, 2, NB])
    e127 = c.sb("e127", [128, 128])
    identf = c.sb("identf", [128, 128])
    ctr = c.sb("ctr", [128, 2, 128])
    biasT = c.sb("biasT", [128, NB, NKT])
    crow = c.sb("crow", [1, BLK])
    crow2 = c.sb("crow2", [1, BLK])
    chi = [c.sb("chi%d" % i, [1, BLK], BF16) for i in range(3)]
    c.psum_banks(8)
    cnt = {"pT": 0, "oout": 0}

    def load(dst, src, key, **k):
        return S.dma(lambda e: e.dma_start(out=dst, in_=src), writes=[key], semkey=key, **k)

    def store(dst, src, rkey):
        return S.dma(lambda e: e.dma_start(out=dst, in_=src), reads=[rkey], semkey="st_" + rkey, final=True)

    load(lamv[:], lam_d, "lamv")
    load(subg[:], subg_d, "subg")
    load(utri[:], utri_d, "utri")
    load(identf[:], ident_d, "identf")
    S.dve(lambda e: e.memset(onesf[:], 1.0), writes=["onesf"])
    S.dve(lambda e: e.memset(ones_b[:], 1.0), writes=["ones_b"])
    for i in range(NS):
        S.pool(lambda e, i=i: e.memset(vt[i][:, :, 64:65], 1.0), writes=["vt%d" % i])
    S.dve(lambda e: e.tensor_tensor(lamv[:, 0, :], lamv[:, 0, :], lamv[:, 1, :], ALU.mult), reads=["lamv"], writes=["lamv"])
    S.dve(lambda e: e.tensor_tensor(lamv[:, 2, :], lamv[:, 2, :], lamv[:, 3, :], ALU.mult), reads=["lamv"], writes=["lamv"])
    S.dve(lambda e: e.reduce_sum(lamt[:, 0:1], lamv[:, 0, :], AX.X), reads=["lamv"], writes=["lamt"])
    S.dve(lambda e: e.reduce_sum(lamt[:, 1:2], lamv[:, 2, :], AX.X), reads=["lamv", "lamt"], writes=["lamt"])
    S.act(lambda e: e.activation(out=lamt[:, 2:4], in_=lamt[:, 0:2], func=AF.Exp), reads=["lamt"], writes=["lamt"])
    S.dve(lambda e: e.tensor_tensor(lamt[:, 4:5], lamt[:, 3:4], lamt[:, 2:3], ALU.subtract), reads=["lamt"], writes=["lamt"])
    S.dve(lambda e: e.tensor_scalar(lamt[:, 4:5], lamt[:, 4:5], -lam_init, None, ALU.add), reads=["lamt"], writes=["lamt"])

    def attend(b, qT, qk, kT, kk, dk, V, vk, scale, causal, bias=None):
        po, pok = c.bank((4, 6))
        nkt = 4 * (b + 1)
        pend = []

        def qk_mm(kt):
            ps, psk = c.bank((0, 4))
            j = kt - 4 * b
            c0 = 128 * j if j > 0 else 0
            S.pe(lambda e: e.matmul(ps[:, c0:BLK], lhsT=kT[0:dk, kt * 128:(kt + 1) * 128], rhs=qT[0:dk, b * BLK + c0:(b + 1) * BLK],
                                    start=True, stop=True), reads=[qk, kk], writes=[psk])
            i = cnt["pT"] % 4
            cnt["pT"] += 1
            p = pT[i]
            pk = "pT%d" % i
            if bias is None:
                S.act(lambda e: e.activation(out=p[:, c0:BLK], in_=ps[:, c0:BLK], func=AF.Exp, scale=scale), reads=[psk], writes=[pk])
            else:
                S.act(lambda e: e.activation(out=p[:, c0:BLK], in_=ps[:, c0:BLK], func=AF.Exp, scale=scale, bias=bias[:, b, kt:kt + 1]),
                      reads=[psk, "biasT"], writes=[pk])
            if j >= 0:
                if causal == "chunk":
                    S.pool(lambda e: e.memset(p[64:128, c0:c0 + 64], 0.0), reads=[pk], writes=[pk])
                else:
                    S.pool(lambda e: e.affine_select(out=p[:, c0:c0 + 128], in_=p[:, c0:c0 + 128], pattern=[[1, 128]],
                                                     compare_op=ALU.is_ge, fill=0.0, base=0, channel_multiplier=-1),
                           reads=[pk], writes=[pk])
            return (kt, p, pk, c0)

        def pv_mm(item):
            kt, p, pk, c0 = item
            S.pe(lambda e: e.matmul(po[0:65, c0:BLK], lhsT=V[:, kt, 0:65], rhs=p[:, c0:BLK], start=(kt == 0), stop=(kt == nkt - 1)),
                 reads=[pk, vk], writes=[pok])

        LOOK = 2
        for kt in range(nkt):
            pend.append(qk_mm(kt))
            if len(pend) > LOOK:
                pv_mm(pend.pop(0))
        while pend:
            pv_mm(pend.pop(0))
        return po, pok

    def recip_bcast(po, pok, slot, mult_ap=None):
        S.dve(lambda e: e.reciprocal(rl[64:65, slot, :], po[64:65, :]), reads=[pok], writes=["rl%d" % slot])
        pr, prk = c.bank((6, 8))
        S.pe(lambda e: e.matmul(pr[0:64, :], lhsT=onesf[64:65, 0:64], rhs=rl[64:65, slot, :], start=True, stop=True),
             reads=["onesf", "rl%d" % slot], writes=[prk])
        return pr, prk

    def emit_out(src_ap, src_keys, row0, b, eng="act"):
        i = cnt["oout"] % 2
        cnt["oout"] += 1
        o = oout[i]
        S.act(lambda e: e.copy(o[:], src_ap), reads=src_keys, writes=["oout%d" % i])
        store(oT[row0:row0 + 64, b * BLK:(b + 1) * BLK], o[:], "oout%d" % i)

    def normalize(po, pok, slot):
        pr, prk = recip_bcast(po, pok, slot)
        S.act(lambda e: e.copy(osb[slot][:], pr[0:64, :]), reads=[prk], writes=["osb%d" % slot])
        S.dve(lambda e: e.tensor_tensor(osb[slot][:], osb[slot][:], po[0:64, :], ALU.mult), reads=["osb%d" % slot, pok], writes=["osb%d" % slot])

    def load_head(slot, q_src, k_src, v_src, d):
        load(qt[slot][0:d, :], q_src, "qt%d" % slot)
        load(kt_[slot][0:d, :], k_src, "kt%d" % slot)
        S.dma(lambda e: e.dma_start(out=vt[slot][:, :, 0:64], in_=v_src.rearrange("(t p) d -> p t d", p=128)),
              writes=["vt%d" % slot], semkey="vt%d" % slot)

    hs = 0
    for h in range(4):
        s_ = hs % NS
        hs += 1
        load_head(s_, qm_d[h], km_d[h], vm_d[:, h * 64:(h + 1) * 64], 96)
        for b in range(NB):
            po, pok = attend(b, qt[s_], "qt%d" % s_, kt_[s_], "kt%d" % s_, 96, vt[s_], "vt%d" % s_, 96.0 ** -0.5, "chunk")
            normalize(po, pok, 0)
            emit_out(osb[0][:], ["osb0"], h * 64, b)
    for h in range(2):
        sl = []
        for m in range(2):
            s_ = hs % NS
            hs += 1
            sl.append(s_)
            load(qt[s_][0:32, :], qd_d[2 * h + m], "qt%d" % s_)
            load(kt_[s_][0:32, :], kd_d[2 * h + m], "kt%d" % s_)
        for s_ in sl:
            S.dma(lambda e, s_=s_, h=h: e.dma_start(out=vt[s_][:, :, 0:64], in_=vd_d[:, h * 64:(h + 1) * 64].rearrange("(t p) d -> p t d", p=128)),
                  writes=["vt%d" % s_], semkey="vt%d" % s_)
        for b in range(NB):
            for m in range(2):
                s_ = sl[m]
                po, pok = attend(b, qt[s_], "qt%d" % s_, kt_[s_], "kt%d" % s_, 32, vt[s_], "vt%d" % s_, 32.0 ** -0.5, "chunk")
                normalize(po, pok, m)
            S.dve(lambda e: e.scalar_tensor_tensor(dsb[:], osb[1][:], lamt[0:64, 4:5], osb[0][:], ALU.mult, ALU.add),
                  reads=["osb0", "osb1", "lamt"], writes=["dsb"])
            S.act(lambda e: e.activation(out=dsq[:], in_=dsb[:], func=AF.Square), reads=["dsb"], writes=["dsq"])
            pr, prk = c.bank((6, 8))
            S.pe(lambda e, pr=pr: e.matmul(pr[0:64, :], lhsT=ones_b[0:64, 0:64], rhs=dsq[:], start=True, stop=True),
                 reads=["ones_b", "dsq"], writes=[prk])
            S.dve(lambda e, pr=pr: e.tensor_scalar(rsd[:], pr[0:64, :], 1.0 / 64, RMS_EPS, ALU.mult, ALU.add), reads=[prk], writes=["rsd"])
            S.act(lambda e: e.activation(out=rsd[:], in_=rsd[:], func=AF.Ln), reads=["rsd"], writes=["rsd"])
            S.act(lambda e: e.activation(out=rsd[:], in_=rsd[:], func=AF.Exp, scale=-0.5), reads=["rsd"], writes=["rsd"])
            S.dve(lambda e: e.tensor_tensor(dsb[:], dsb[:], rsd[:], ALU.mult), reads=["dsb", "rsd"], writes=["dsb"])
            S.dve(lambda e: e.tensor_scalar(dsb[:], dsb[:], subg[:, 0:1], 1.0 - lam_init, ALU.mult, ALU.mult), reads=["dsb", "subg"], writes=["dsb"])
            emit_out(dsb[:], ["dsb"], 256 + h * 64, b)
    S.dma(lambda e: e.dma_start(out=lft[:], in_=lf_d.rearrange("(t p) h -> p t h", p=128)), writes=["lft"], semkey="lft")
    S.dve(lambda e: e.memset(carry[:], 0.0), writes=["carry"])
    S.pool(lambda e: e.memset(e127[:], 1.0), writes=["e127"])
    S.pool(lambda e: e.affine_select(out=e127[:], in_=e127[:], pattern=[[0, 128]], compare_op=ALU.is_ge, fill=0.0,
                                     base=-127, channel_multiplier=1), reads=["e127"], writes=["e127"])
    for t in range(NKT):
        pc, pck = c.bank((6, 8))
        S.pe(lambda e, pc=pc, t=t: e.matmul(pc[:, 0:2], lhsT=utri[:], rhs=lft[:, t, :], start=True, stop=True), reads=["utri", "lft"], writes=[pck])
        S.pe(lambda e, pc=pc, t=t: e.matmul(pc[:, 2:4], lhsT=onesf[:], rhs=lft[:, t, :], start=True, stop=True), reads=["onesf", "lft"], writes=[pck])
        S.dve(lambda e, pc=pc, t=t: e.tensor_tensor(cumT[:, :, t], pc[:, 0:2], carry[:], ALU.add), reads=[pck, "carry"], writes=["cumT"])
        S.dve(lambda e, pc=pc: e.tensor_tensor(carry[:], carry[:], pc[:, 2:4], ALU.add), reads=[pck, "carry"], writes=["carry"])
    S.dve(lambda e: e.memset(csel[:], 0.0), writes=["csel"])
    for b in range(1, NB):
        S.dve(lambda e, b=b: e.tensor_copy(csel[:, :, b], cumT[:, :, 4 * b - 1]), reads=["cumT", "csel"], writes=["csel"])
    pc, pck = c.bank((6, 8))
    S.pe(lambda e: e.matmul(pc[:, 0:2 * NB], lhsT=e127[:], rhs=csel[:].rearrange("p h b -> p (h b)"), start=True, stop=True),
         reads=["e127", "csel"], writes=[pck])
    S.dve(lambda e: e.tensor_copy(cb[:].rearrange("p h b -> p (h b)"), pc[:, 0:2 * NB]), reads=[pck], writes=["cb"])
    cs = []
    for h in range(2):
        pc, pck = c.bank((6, 8))
        S.pe(lambda e, pc=pc, h=h: e.transpose(pc[0:NKT, 0:128], cumT[:, h, :], identf[:]), reads=["cumT", "identf"], writes=[pck])
        S.dve(lambda e, pc=pc, h=h: e.tensor_copy(ctr[0:NKT, h, :], pc[0:NKT, 0:128]), reads=[pck], writes=["ctr%d" % h])
        cs.append(S.dma(lambda e, h=h: e.dma_start(out=cum_scr[h].rearrange("(t p) -> t p", p=128), in_=ctr[0:NKT, h, :]),
                        reads=["ctr%d" % h], semkey="cumscr_w%d" % h))
    for h in range(2):
        s_ = hs % NS
        hs += 1
        load_head(s_, qf_d[h], kf_d[h], vf_d[:, h * 64:(h + 1) * 64], 64)
        for b in range(NB):
            n = 4 * (b + 1)
            S.dve(lambda e, b=b, n=n, h=h: e.tensor_scalar(biasT[:, b, 0:n], cumT[:, h, 0:n], cb[:, h, b:b + 1], -1.0, ALU.subtract, ALU.mult),
                  reads=["cumT", "cb"], writes=["biasT"])
        for b in range(NB):
            S.dma(lambda e, h=h, b=b: e.dma_start(out=crow[:], in_=cum_scr[h:h + 1, b * BLK:(b + 1) * BLK]), writes=["crow"], semkey="crow", extra=cs)
            S.dve(lambda e, h=h, b=b: e.tensor_scalar(crow[:], crow[:], cb[0:1, h, b:b + 1], 8.0, ALU.subtract, ALU.mult), reads=["crow", "cb"], writes=["crow"])
            for r in range(3):
                ch = chi[r]
                S.dve(lambda e, ch=ch: e.tensor_copy(ch[:], crow[:]), reads=["crow"], writes=["chi%d" % r])
                S.dma(lambda e, s_=s_, r=r, b=b, ch=ch: e.dma_start(out=qt[s_][64 + r:65 + r, b * BLK:(b + 1) * BLK], in_=ch[:]),
                      reads=["chi%d" % r], writes=["qt%d" % s_], semkey="chi%d" % r)
                if r < 2:
                    S.dve(lambda e, ch=ch: e.tensor_copy(crow2[:], ch[:]), reads=["chi%d" % r], writes=["crow2"])
                    S.dve(lambda e: e.tensor_tensor(crow[:], crow[:], crow2[:], ALU.subtract), reads=["crow", "crow2"], writes=["crow"])
        S.pool(lambda e, s_=s_: e.memset(kt_[s_][64:67, :], 1.0), reads=["kt%d" % s_], writes=["kt%d" % s_])
        for b in range(NB):
            po, pok = attend(b, qt[s_], "qt%d" % s_, kt_[s_], "kt%d" % s_, 67, vt[s_], "vt%d" % s_, 0.125, "frame", bias=biasT)
            normalize(po, pok, 0)
            emit_out(osb[0][:], ["osb0"], 384 + h * 64, b)
    return c.finish()


def build_C(T, SBT=1024):
    c = Ctx()
    S = c.S
    NSB = T // SBT
    NT = SBT // 128
    NBK = SBT // BLK
    oT_d = c.din("oT", [128, 8, T], BF16)
    h_d = c.din("hres", [T, D])
    wout_d = c.din("w_out", [128, 8, D])
    l1g_d = c.din("ln1g", [128, D])
    l1b_d = c.din("ln1b", [128, D])
    l2g_d = c.din("ln2g", [128, D])
    l2b_d = c.din("ln2b", [128, D])
    rw_d = c.din("router_w", [128, 8, 16])
    rb_d = c.din("router_b", [128, 16])
    wg_d = c.din("wg", [16, 128, 8, 512])
    wu_d = c.din("wu", [16, 128, 8, 512])
    wd_d = c.din("wd", [16, 128, 4, D])
    ident_d = c.din("ident", [128, 128])
    hout = c.dout("hout", [T, D])

    wout = c.sb("wout", [128, 8, D], BF16)
    wst = c.sb("wst", [128, D])
    l1g = c.sb("l1g", [128, D]); l1b = c.sb("l1b", [128, D]); l2g = c.sb("l2g", [128, D]); l2b = c.sb("l2b", [128, D])
    rw = c.sb("rw", [128, 8, 16]); rb = c.sb("rb", [128, 16])
    identf = c.sb("identf", [128, 128]); identb = c.sb("identb", [128, 128], BF16)
    h1 = c.sb("h1", [128, NT, D])
    yacc = c.sb("yacc", [128, NT, D])
    h1T = c.sb("h1T", [128, 8, SBT], BF16)
    gates = c.sb("gates", [128, NT, 16])
    wg = [c.sb("wg%d" % i, [128, 8, 512], BF16) for i in range(2)]
    wu = [c.sb("wu%d" % i, [128, 8, 512], BF16) for i in range(2)]
    wd = [c.sb("wd%d" % i, [128, 4, D], BF16) for i in range(2)]
    ot = [c.sb("ot%d" % i, [128, 8, 128], BF16) for i in range(2)]
    ht = [c.sb("ht%d" % i, [128, D]) for i in range(2)]
    hb = c.sb("hb", [128, D], BF16)
    hTf = c.sb("hTf", [128, 8, 128])
    junk = c.sb("junk", [128, D], BF16)
    stat = c.sb("stat", [128, 8])
    sg = c.sb("sg", [128, BLK])
    aT = [c.sb("aT%d" % i, [128, BLK], BF16) for i in range(4)]
    sc = c.sb("sc", [128, 16]); bz = c.sb("bz", [128, 16]); prs = c.sb("prs", [128, 4, 6]); gsc = c.sb("gsc", [128, 4])
    gmx = c.sb("gmx", [128, 4]); cand = c.sb("cand", [128, 16]); msk = c.sb("msk", [128, 16]); msk2 = c.sb("msk2", [128, 16])
    c.psum_banks(7)
    pst = c.st.enter_context(c.nc.psum_tensor("pstb", [128, 1024], BF16))
    PA, PB, PC = (0, 4), (4, 6), (6, 7)
    cnt = {"ot": 0, "ht": 0, "aT": 0}
    BIG = 1.0e4

    def load(dst, src, key, **k):
        return S.dma(lambda e: e.dma_start(out=dst, in_=src), writes=[key], semkey=key, **k)

    load(l1g[:], l1g_d, "l1g"); load(l1b[:], l1b_d, "l1b"); load(l2g[:], l2g_d, "l2g"); load(l2b[:], l2b_d, "l2b")
    load(rw[:], rw_d, "rw"); load(rb[:], rb_d, "rb"); load(identf[:], ident_d, "identf")
    S.dve(lambda e: e.tensor_copy(identb[:], identf[:]), reads=["identf"], writes=["identb"])
    for k in range(8):
        load(wst[:], wout_d[:, k, :], "wst")
        S.dve(lambda e, k=k: e.tensor_copy(wout[:, k, :], wst[:]), reads=["wst"], writes=["wout"])

    def layernorm(x, xk, g, gk, b, bk):
        S.act(lambda e: e.activation(out=junk[:], in_=x, func=AF.Identity, accum_out=stat[:, 0:1]), reads=[xk], writes=["junk", "stat"])
        S.act(lambda e: e.activation(out=junk[:], in_=x, func=AF.Square, accum_out=stat[:, 1:2]), reads=[xk], writes=["junk", "stat"])
        S.dve(lambda e: e.tensor_scalar(stat[:, 2:4], stat[:, 0:2], 1.0 / D, None, ALU.mult), reads=["stat"], writes=["stat"])
        S.dve(lambda e: e.tensor_tensor(stat[:, 4:5], stat[:, 2:3], stat[:, 2:3], ALU.mult), reads=["stat"], writes=["stat"])
        S.dve(lambda e: e.tensor_tensor(stat[:, 5:6], stat[:, 3:4], stat[:, 4:5], ALU.subtract), reads=["stat"], writes=["stat"])
        S.dve(lambda e: e.tensor_scalar(stat[:, 6:7], stat[:, 5:6], LN_EPS, None, ALU.add), reads=["stat"], writes=["stat"])
        S.act(lambda e: e.activation(out=stat[:, 6:7], in_=stat[:, 6:7], func=AF.Ln), reads=["stat"], writes=["stat"])
        S.act(lambda e: e.activation(out=stat[:, 6:7], in_=stat[:, 6:7], func=AF.Exp, scale=-0.5), reads=["stat"], writes=["stat"])
        S.dve(lambda e: e.tensor_scalar(x, x, stat[:, 2:3], stat[:, 6:7], ALU.subtract, ALU.mult), reads=[xk, "stat"], writes=[xk])
        S.dve(lambda e: e.tensor_tensor(x, x, g[:], ALU.mult), reads=[xk, gk], writes=[xk])
        S.dve(lambda e: e.tensor_tensor(x, x, b[:], ALU.add), reads=[xk, bk], writes=[xk])

    def load_expert(e_, slot):
        S.dma(lambda e: e.dma_start(out=wg[slot][:], in_=wg_d[e_]), writes=["wg%d" % slot], semkey="wg%d" % slot, eng="pool")
        S.dma(lambda e: e.dma_start(out=wu[slot][:], in_=wu_d[e_]), writes=["wu%d" % slot], semkey="wu%d" % slot, eng="pool")
        S.dma(lambda e: e.dma_start(out=wd[slot][:], in_=wd_d[e_]), writes=["wd%d" % slot], semkey="wd%d" % slot, eng="pool")

    for sb_ in range(NSB):
        tb = sb_ * SBT
        for tt in range(NT):
            r0 = tb + tt * 128
            i = cnt["ot"] % 2
            cnt["ot"] += 1
            o_, ok = ot[i], "ot%d" % i
            load(o_[:], oT_d[:, :, r0:r0 + 128], ok)
            x_, xk = ht[i], "ht%d" % i
            load(x_[:], h_d[r0:r0 + 128, :], xk)
            hk = "h1_%d" % tt
            for half in range(2):
                pb, pk = c.bank(PA)
                for k in range(8):
                    S.pe(lambda e, pb=pb, k=k, o_=o_, half=half: e.matmul(pb[:, :], lhsT=o_[:, k, :], rhs=wout[:, k, half * 512:(half + 1) * 512],
                                                                       start=(k == 0), stop=(k == 7)), reads=[ok, "wout"], writes=[pk])
                S.dve(lambda e, pb=pb, half=half, x_=x_, tt=tt: e.scalar_tensor_tensor(h1[:, tt, half * 512:(half + 1) * 512], x_[:, half * 512:(half + 1) * 512],
                                                                                      ALPHA, pb[:, :], ALU.mult, ALU.add),
                      reads=[xk, pk], writes=[hk])
            layernorm(h1[:, tt, :], hk, l1g, "l1g", l1b, "l1b")
            S.act(lambda e, tt=tt: e.copy(hb[:], h1[:, tt, :]), reads=[hk], writes=["hb"])
            for k in range(8):
                S.pe(lambda e, k=k: e.transpose(pst[:, k * 128:(k + 1) * 128], hb[:, k * 128:(k + 1) * 128], identb[:]),
                     reads=["hb", "identb"], writes=["pstb"])
            S.act(lambda e, tt=tt: e.copy(h1T[:, :, tt * 128:(tt + 1) * 128], pst[:, :].rearrange("p (k n) -> p k n", n=128)),
                  reads=["pstb"], writes=["h1T"])
            for half in range(2):
                pb, pk = c.bank(PA)
                for kk in range(4):
                    k = half * 4 + kk
                    S.pe(lambda e, pb=pb, k=k, kk=kk, tt=tt: e.transpose(pb[:, kk * 128:(kk + 1) * 128], h1[:, tt, k * 128:(k + 1) * 128], identf[:]),
                         reads=[hk, "identf"], writes=[pk])
                S.dve(lambda e, pb=pb, half=half: e.tensor_copy(hTf[:, half * 4:(half + 1) * 4, :], pb[:, :].rearrange("p (k n) -> p k n", n=128)),
                      reads=[pk], writes=["hTf%d" % half])
            pl, plk = c.bank(PC)
            for k in range(8):
                S.pe(lambda e, pl=pl, k=k: e.matmul(pl[:, 0:16], lhsT=hTf[:, k, :], rhs=rw[:, k, :], start=(k == 0), stop=(k == 7)),
                     reads=["hTf0", "hTf1", "rw"], writes=[plk])
            R = ["sc", "bz", "prs", "gsc", "gmx", "cand", "msk", "msk2"]
            S.act(lambda e, pl=pl: e.activation(out=sc[:], in_=pl[:, 0:16], func=AF.Sigmoid), reads=[plk], writes=["sc"])
            S.dve(lambda e: e.tensor_tensor(bz[:], sc[:], rb[:], ALU.add), reads=["sc", "rb"], writes=["bz"])
            bz3 = bz[:].rearrange("p (g j) -> p g j", j=4)
            pi = 0
            for a in range(4):
                for b_ in range(a + 1, 4):
                    S.dve(lambda e, a=a, b_=b_, pi=pi: e.tensor_tensor(prs[:, :, pi], bz3[:, :, a], bz3[:, :, b_], ALU.add), reads=["bz"], writes=["prs"])
                    pi += 1
            S.dve(lambda e: e.tensor_reduce(gsc[:], prs[:], AX.X, ALU.max), reads=["prs"], writes=["gsc"])
            S.dve(lambda e: e.tensor_reduce(gmx[:, 0:1], gsc[:], AX.X, ALU.max), reads=["gsc"], writes=["gmx"])
            S.dve(lambda e: e.tensor_scalar(gsc[:], gsc[:], gmx[:, 0:1], None, ALU.is_equal), reads=["gsc", "gmx"], writes=["gsc"])
            S.dve(lambda e: e.tensor_scalar(gsc[:], gsc[:], BIG, -BIG, ALU.mult, ALU.add), reads=["gsc"], writes=["gsc"])
            S.dve(lambda e: e.tensor_tensor(cand[:].rearrange("p (g j) -> p g j", j=4), bz3, gsc[:, :, None].to_broadcast([128, 4, 4]), ALU.add),
                  reads=["bz", "gsc"], writes=["cand"])
            S.dve(lambda e: e.tensor_reduce(gmx[:, 1:2], cand[:], AX.X, ALU.max), reads=["cand"], writes=["gmx"])
            S.dve(lambda e: e.tensor_scalar(msk[:], cand[:], gmx[:, 1:2], None, ALU.is_equal), reads=["cand", "gmx"], writes=["msk"])
            S.dve(lambda e: e.scalar_tensor_tensor(cand[:], msk[:], -BIG, cand[:], ALU.mult, ALU.add), reads=["cand", "msk"], writes=["cand"])
            S.dve(lambda e: e.tensor_reduce(gmx[:, 2:3], cand[:], AX.X, ALU.max), reads=["cand"], writes=["gmx"])
            S.dve(lambda e: e.tensor_scalar(msk2[:], cand[:], gmx[:, 2:3], None, ALU.is_equal), reads=["cand", "gmx"], writes=["msk2"])
            S.dve(lambda e: e.tensor_tensor(msk[:], msk[:], msk2[:], ALU.add), reads=["msk", "msk2"], writes=["msk"])
            S.dve(lambda e: e.tensor_tensor(msk[:], msk[:], sc[:], ALU.mult), reads=["msk", "sc"], writes=["msk"])
            S.dve(lambda e: e.tensor_reduce(gmx[:, 3:4], msk[:], AX.X, ALU.add), reads=["msk"], writes=["gmx"])
            S.dve(lambda e: e.reciprocal(gmx[:, 3:4], gmx[:, 3:4]), reads=["gmx"], writes=["gmx"])
            S.dve(lambda e, tt=tt: e.tensor_scalar(gates[:, tt, :], msk[:], gmx[:, 3:4], None, ALU.mult), reads=["msk", "gmx"], writes=["gates"])
        S.pool(lambda e: e.memset(yacc[:], 0.0), writes=["yacc%d" % t for t in range(NT)])
        if sb_ == 0:
            load_expert(0, 0)
        for ex in range(16):
            sl = ex % 2
            nxt = ex + 1
            if nxt < 16:
                load_expert(nxt, nxt % 2)
            elif sb_ + 1 < NSB:
                load_expert(0, 0)
            WK = ["wg%d" % sl, "wu%d" % sl]
            for bk in range(NBK):
                cs_ = bk * BLK
                ats = []
                for fi in range(4):
                    pg, pgk = c.bank(PA)
                    pu, puk = c.bank(PA)
                    for k in range(8):
                        S.pe(lambda e, pg=pg, k=k, fi=fi, sl=sl, cs_=cs_: e.matmul(pg[:, :], lhsT=wg[sl][:, k, fi * 128:(fi + 1) * 128], rhs=h1T[:, k, cs_:cs_ + BLK],
                                                                                 start=(k == 0), stop=(k == 7)), reads=WK + ["h1T"], writes=[pgk])
                    for k in range(8):
                        S.pe(lambda e, pu=pu, k=k, fi=fi, sl=sl, cs_=cs_: e.matmul(pu[:, :], lhsT=wu[sl][:, k, fi * 128:(fi + 1) * 128], rhs=h1T[:, k, cs_:cs_ + BLK],
                                                                                 start=(k == 0), stop=(k == 7)), reads=WK + ["h1T"], writes=[puk])
                    S.act(lambda e, pg=pg: e.activation(out=sg[:], in_=pg[:, :], func=AF.Silu), reads=[pgk], writes=["sg"])
                    ai = cnt["aT"] % 4
                    cnt["aT"] += 1
                    S.dve(lambda e, pu=pu, ai=ai: e.tensor_tensor(aT[ai][:], sg[:], pu[:, :], ALU.mult), reads=["sg", puk], writes=["aT%d" % ai])
                    ats.append(ai)
                for t4 in range(4):
                    tt = bk * 4 + t4
                    for half in range(2):
                        py, pyk = c.bank(PB)
                        for fi in range(4):
                            S.pe(lambda e, py=py, fi=fi, ai=ats[fi], t4=t4, half=half, sl=sl: e.matmul(py[:, :], lhsT=aT[ai][:, t4 * 128:(t4 + 1) * 128],
                                                                                                   rhs=wd[sl][:, fi, half * 512:(half + 1) * 512], start=(fi == 0), stop=(fi == 3)),
                                 reads=["aT%d" % ats[fi], "wd%d" % sl], writes=[pyk])
                        S.dve(lambda e, py=py, tt=tt, half=half, ex=ex: e.scalar_tensor_tensor(yacc[:, tt, half * 512:(half + 1) * 512], py[:, :], gates[:, tt, ex:ex + 1],
                                                                                             yacc[:, tt, half * 512:(half + 1) * 512], ALU.mult, ALU.add),
                              reads=[pyk, "gates", "yacc%d" % tt], writes=["yacc%d" % tt])
        for tt in range(NT):
            r0 = tb + tt * 128
            S.dve(lambda e, tt=tt: e.scalar_tensor_tensor(yacc[:, tt, :], h1[:, tt, :], ALPHA, yacc[:, tt, :], ALU.mult, ALU.add),
                  reads=["h1_%d" % tt, "yacc%d" % tt], writes=["yacc%d" % tt])
            layernorm(yacc[:, tt, :], "yacc%d" % tt, l2g, "l2g", l2b, "l2b")
            S.dma(lambda e, tt=tt, r0=r0: e.dma_start(out=hout[r0:r0 + 128, :], in_=yacc[:, tt, :]), reads=["yacc%d" % tt], semkey="st_yacc%d" % tt, final=True)
    return c.finish()


def weights_C(inp, l):
    return {
        "w_out": kchunk(inp["w_out"][l], 8),
        "ln1g": bcast_rows(inp["ln1_g"][l]), "ln1b": bcast_rows(inp["ln1_b"][l]),
        "ln2g": bcast_rows(inp["ln2_g"][l]), "ln2b": bcast_rows(inp["ln2_b"][l]),
        "router_w": kchunk(inp["router_w"], 8), "router_b": bcast_rows(inp["router_bias"]),
        "wg": np.ascontiguousarray(inp["exp_w_gate"][l].reshape(16, 8, 128, 512).transpose(0, 2, 1, 3)),
        "wu": np.ascontiguousarray(inp["exp_w_up"][l].reshape(16, 8, 128, 512).transpose(0, 2, 1, 3)),
        "wd": np.ascontiguousarray(inp["exp_w_down"][l].reshape(16, 4, 128, 1024).transpose(0, 2, 1, 3)),
        "ident": np.eye(128, dtype=np.float32),
    }


NCORES = 8
_PROGS = {}


def _prog(key, builder):
    if key not in _PROGS:
        _PROGS[key] = builder()
    return _PROGS[key]


def _run(nc, maps):
    return run_bass_kernel_spmd(nc, maps, core_ids=list(range(NCORES))).results


def kernel(x, positions, ln_in_g, ln_in_b, w_in, mla_q_norm_g, mla_kv_norm_g, mla_w_uq, mla_w_ukv,
           diff_lam_q1, diff_lam_k1, diff_lam_q2, diff_lam_k2, diff_subln_g, fox_f_bias, w_out,
           ln1_g, ln1_b, router_w, router_bias, exp_w_gate, exp_w_up, exp_w_down, ln2_g, ln2_b):
    inp = dict(w_in=w_in, mla_q_norm_g=mla_q_norm_g, mla_kv_norm_g=mla_kv_norm_g, mla_w_uq=mla_w_uq, mla_w_ukv=mla_w_ukv,
               fox_f_bias=fox_f_bias, w_out=w_out, ln1_g=ln1_g, ln1_b=ln1_b, router_w=router_w, router_bias=router_bias,
               exp_w_gate=exp_w_gate, exp_w_up=exp_w_up, exp_w_down=exp_w_down, ln2_g=ln2_g, ln2_b=ln2_b)
    inp = {k: np.asarray(v, np.float32) for k, v in inp.items()}
    x = np.asarray(x, np.float32)
    positions = np.asarray(positions, np.int32)
    B_, S_, _ = x.shape
    depth = w_in.shape[0]
    T = S_ // 2
    utri = np.triu(np.ones((128, 128), np.float32))
    ident = np.eye(128, dtype=np.float32)
    h = [x[c // 2, (c % 2) * T:(c % 2 + 1) * T] for c in range(NCORES)]
    posr = [np.ascontiguousarray(np.broadcast_to(positions[c // 2, (c % 2) * T:(c % 2 + 1) * T][None, :], (128, T))) for c in range(NCORES)]
    for l in range(depth):
        lam_init = 0.8 - 0.6 * math.exp(-0.3 * l)
        first = (l == 0)
        WA = weights_A(inp, l)
        if first:
            WA["lng"] = bcast_rows(np.asarray(ln_in_g, np.float32))
            WA["lnb"] = bcast_rows(np.asarray(ln_in_b, np.float32))
        ncA = _prog(("A", T, first), lambda: build_A(T, first))
        ra = _run(ncA, [dict(WA, hin=np.ascontiguousarray(h[c]), posr=posr[c]) for c in range(NCORES)])
        if first:
            h = [ra[c]["hres"] for c in range(NCORES)]
        mapsB = []
        lamv = np.ascontiguousarray(np.broadcast_to(np.stack([np.asarray(a, np.float32)[l] for a in
                                   (diff_lam_q1, diff_lam_k1, diff_lam_q2, diff_lam_k2)])[None], (128, 4, 32)))
        subg = np.ascontiguousarray(np.asarray(diff_subln_g, np.float32)[l].reshape(64, 1))
        for b in range(B_):
            cat = lambda name, ax: np.concatenate([ra[2 * b][name], ra[2 * b + 1][name]], axis=ax)
            qm = cat("qm", 2)
            kn = cat("kmn", 2).reshape(8, 64, S_)
            kr = cat("kr", 1)
            km = np.concatenate([kn, np.broadcast_to(kr[None], (8, 32, S_))], axis=1)
            qd = cat("qd", 2).reshape(8, 32, S_)
            kd = cat("kd", 2).reshape(8, 32, S_)
            qf = cat("qf", 2).reshape(4, 64, S_)
            kf = cat("kf", 2).reshape(4, 64, S_)
            vm = cat("vm", 0)
            vdf = cat("vdf", 0)
            lf = cat("lf", 0)
            for g in range(2):
                mapsB.append(dict(
                    qm=np.ascontiguousarray(qm[4 * g:4 * g + 4]), km=np.ascontiguousarray(km[4 * g:4 * g + 4]),
                    vm=np.ascontiguousarray(vm[:, 256 * g:256 * (g + 1)]),
                    qd=np.ascontiguousarray(qd[4 * g:4 * g + 4]), kd=np.ascontiguousarray(kd[4 * g:4 * g + 4]),
                    vd=np.ascontiguousarray(vdf[:, 128 * g:128 * (g + 1)]),
                    qf=np.ascontiguousarray(qf[2 * g:2 * g + 2]), kf=np.ascontiguousarray(kf[2 * g:2 * g + 2]),
                    vf=np.ascontiguousarray(vdf[:, 256 + 128 * g:256 + 128 * (g + 1)]),
                    lf=np.ascontiguousarray(lf[:, 2 * g:2 * g + 2]),
                    lamv=lamv, subg=subg, utri=utri, ident=ident))
        ncB = _prog(("B", S_, round(lam_init, 9)), lambda: build_B(S_, lam_init))
        rb = _run(ncB, mapsB)
        WC = weights_C(inp, l)
        mapsC = []
        for c in range(NCORES):
            b, g = c // 2, c % 2
            o0, o1 = rb[2 * b]["oT"], rb[2 * b + 1]["oT"]
            of = np.concatenate([o0[0:256], o1[0:256], o0[256:384], o1[256:384], o0[384:512], o1[384:512]], axis=0)
            ot = np.ascontiguousarray(of[:, g * T:(g + 1) * T].reshape(8, 128, T).transpose(1, 0, 2))
            mapsC.append(dict(WC, oT=ot, hres=np.ascontiguousarray(h[c])))
        ncC = _prog(("C", T), lambda: build_C(T, min(1024, T)))
        rc = _run(ncC, mapsC)
        h = [rc[c]["hout"] for c in range(NCORES)]
    out = np.empty((B_, S_, D), np.float32)
    for c in range(NCORES):
        out[c // 2, (c % 2) * T:(c % 2 + 1) * T] = h[c]
    return out
```
